# Optimizing a Trainium2 kernel written in Bass

```python
import jax, jax.numpy as jnp
from jax import lax
import numpy as np

D_MODEL = 2048
BATCH = 4
SEQ = 2048
DEPTH = 2

ATTN_HEADS = 8
HEAD_DIM = 128
ATTN_WIDTH = ATTN_HEADS * HEAD_DIM
MOBA_BLOCK = 256
MOBA_TOPK = 3
Q_CHUNK = 16
SGU_GROUPS = 8
SGU_GROUP_DIM = 128
SGU_WIDTH = SGU_GROUPS * SGU_GROUP_DIM
SGU_CHUNK = 128
D_FF = 5632
N_EXPERTS = 8
TOP_K = 2
D_FF_EXPERT = 5632
N_DENSE = (DEPTH + 1) // 2
N_MOE = DEPTH // 2
NORM_EPS = 1e-6
IN_SPLITS = (ATTN_WIDTH, ATTN_WIDTH, ATTN_WIDTH, SGU_WIDTH, SGU_WIDTH, D_MODEL, D_MODEL)
IN_WIDTH = sum(IN_SPLITS)

kernel_name = "hybrid_moba_sgu_moe_block"


def rmsnorm(x, g):
    xf = x.astype(jnp.float32)
    y = xf * lax.rsqrt(jnp.mean(xf * xf, axis=-1, keepdims=True) + NORM_EPS) * g.astype(jnp.float32)
    return y.astype(x.dtype)


def layernorm(x, g):
    xf = x.astype(jnp.float32)
    mu = jnp.mean(xf, axis=-1, keepdims=True)
    xc = xf - mu
    y = xc * lax.rsqrt(jnp.mean(xc * xc, axis=-1, keepdims=True) + NORM_EPS) * g.astype(jnp.float32)
    return y.astype(x.dtype)


def moba_attention(q, k, v):
    B, S, H, Dh = q.shape
    nb = -(-S // MOBA_BLOCK)
    sp = nb * MOBA_BLOCK
    pad = ((0, 0), (0, sp - S), (0, 0), (0, 0))
    q, k, v = [jnp.pad(t, pad).transpose(0, 2, 1, 3) for t in (q, k, v)]
    scale = Dh ** -0.5
    kb = k.reshape(B, H, nb, MOBA_BLOCK, Dh)
    vb = v.reshape(B, H, nb, MOBA_BLOCK, Dh)
    k_mean = jnp.mean(kb.astype(jnp.float32), axis=3)
    gate = jnp.einsum('bhsd,bhnd->bhsn', q.astype(jnp.float32), k_mean)
    q_blk = jnp.arange(sp) // MOBA_BLOCK
    past = jnp.arange(nb)[None, :] < q_blk[:, None]
    gate = jnp.where(past, gate, -jnp.inf)
    k_sel = min(MOBA_TOPK, nb)
    _, sel = lax.top_k(gate, k_sel)
    sel_valid = sel < q_blk[:, None]
    b_ix = jnp.arange(B)[:, None, None, None]
    h_ix = jnp.arange(H)[None, :, None, None]

    def attend_chunk(c):
        start = c * Q_CHUNK
        qc = lax.dynamic_slice_in_dim(q, start, Q_CHUNK, axis=2)
        sc = lax.dynamic_slice_in_dim(sel, start, Q_CHUNK, axis=2)
        vc = lax.dynamic_slice_in_dim(sel_valid, start, Q_CHUNK, axis=2)
        kg = kb[b_ix, h_ix, sc]
        vg = vb[b_ix, h_ix, sc]
        s_sel = jnp.einsum('bhqd,bhqkjd->bhqkj', qc, kg).astype(jnp.float32) * scale
        s_sel = jnp.where(vc[..., None], s_sel, -jnp.inf).reshape(B, H, Q_CHUNK, k_sel * MOBA_BLOCK)
        own = start // MOBA_BLOCK
        ko = lax.dynamic_slice_in_dim(k, own * MOBA_BLOCK, MOBA_BLOCK, axis=2)
        vo = lax.dynamic_slice_in_dim(v, own * MOBA_BLOCK, MOBA_BLOCK, axis=2)
        s_own = jnp.einsum('bhqd,bhjd->bhqj', qc, ko).astype(jnp.float32) * scale
        q_pos = start + jnp.arange(Q_CHUNK)
        k_pos = own * MOBA_BLOCK + jnp.arange(MOBA_BLOCK)
        s_own = jnp.where(k_pos[None, :] <= q_pos[:, None], s_own, -jnp.inf)
        p = jax.nn.softmax(jnp.concatenate([s_sel, s_own], axis=-1), axis=-1)
        p_sel = p[..., :k_sel * MOBA_BLOCK].reshape(B, H, Q_CHUNK, k_sel, MOBA_BLOCK).astype(v.dtype)
        p_own = p[..., k_sel * MOBA_BLOCK:].astype(v.dtype)
        return (jnp.einsum('bhqkj,bhqkjd->bhqd', p_sel, vg)
                + jnp.einsum('bhqj,bhjd->bhqd', p_own, vo))

    outs = lax.map(attend_chunk, jnp.arange(sp // Q_CHUNK))
    out = outs.transpose(1, 0, 3, 2, 4).reshape(B, sp, H, Dh)
    return out[:, :S]


def spatial_gating(u, vg, g_sgu, w_s, b_s):
    B, S, _ = u.shape
    u = jax.nn.gelu(u)
    vn = layernorm(jax.nn.gelu(vg), g_sgu)
    nc = S // SGU_CHUNK
    vr = vn.reshape(B, nc, SGU_CHUNK, SGU_GROUPS, SGU_GROUP_DIM)
    mask = jnp.tril(jnp.ones((SGU_CHUNK, SGU_CHUNK), dtype=bool))
    w = jnp.where(mask, w_s, jnp.zeros_like(w_s))
    mixed = jnp.einsum('gts,bcsgd->bctgd', w, vr) + b_s.T[None, None, :, :, None]
    return u * mixed.reshape(B, S, SGU_WIDTH)


def token_mixer(h, w_in, g_sgu, w_s, b_s, w_pa, w_pb, w_o):
    B, S, _ = h.shape
    z = h @ w_in
    offs = np.cumsum(IN_SPLITS)[:-1].tolist()
    q, k, v, u, vg, ga, gb = jnp.split(z, offs, axis=-1)
    shp = (B, S, ATTN_HEADS, HEAD_DIM)
    attn = moba_attention(q.reshape(shp), k.reshape(shp), v.reshape(shp)).reshape(B, S, ATTN_WIDTH)
    sgu = spatial_gating(u, vg, g_sgu, w_s, b_s)
    merged = jax.nn.sigmoid(ga) * (attn @ w_pa) + jax.nn.sigmoid(gb) * (sgu @ w_pb)
    return merged @ w_o


def swiglu(t, wg, wu, wd):
    return (jax.nn.silu(t @ wg) * (t @ wu)) @ wd


def moe_swiglu(h, w_router, b_router, wg, wu, wd):
    B, S, D = h.shape
    t = h.reshape(B * S, D)
    logits = (t @ w_router).astype(jnp.float32) + b_router.astype(jnp.float32)
    top_val, top_idx = lax.top_k(logits, TOP_K)
    top_w = jax.nn.softmax(top_val, axis=-1)
    combine = jnp.sum(jax.nn.one_hot(top_idx, N_EXPERTS, dtype=jnp.float32) * top_w[..., None], axis=1)
    combine = combine.astype(t.dtype)
    out = jnp.zeros_like(t)
    for e in range(N_EXPERTS):
        out = out + combine[:, e:e + 1] * swiglu(t, wg[e], wu[e], wd[e])
    return out.reshape(B, S, D)


def setup_inputs(seed: int = 0) -> dict:
    key = jax.random.key(seed)
    ks = jax.random.split(key, 24)
    f32 = jnp.float32
    nrm = lambda k, shape, s: jax.random.normal(k, shape, f32) * s
    return {
        "x": nrm(ks[0], (BATCH, SEQ, D_MODEL), 1.0),
        "mix_norm_g": 1.0 + nrm(ks[1], (DEPTH, D_MODEL), 0.02),
        "w_in": nrm(ks[2], (DEPTH, D_MODEL, IN_WIDTH), D_MODEL ** -0.5),
        "sgu_norm_g": 1.0 + nrm(ks[3], (DEPTH, SGU_WIDTH), 0.02),
        "w_s": nrm(ks[4], (DEPTH, SGU_GROUPS, SGU_CHUNK, SGU_CHUNK), SGU_CHUNK ** -0.5),
        "b_s": 1.0 + nrm(ks[5], (DEPTH, SGU_GROUPS, SGU_CHUNK), 0.01),
        "w_pa": nrm(ks[6], (DEPTH, ATTN_WIDTH, D_MODEL), ATTN_WIDTH ** -0.5),
        "w_pb": nrm(ks[7], (DEPTH, SGU_WIDTH, D_MODEL), SGU_WIDTH ** -0.5),
        "w_o": nrm(ks[8], (DEPTH, D_MODEL, D_MODEL), D_MODEL ** -0.5),
        "ffn_norm_g": 1.0 + nrm(ks[9], (DEPTH, D_MODEL), 0.02),
        "dense_w_gate": nrm(ks[10], (N_DENSE, D_MODEL, D_FF), D_MODEL ** -0.5),
        "dense_w_up": nrm(ks[11], (N_DENSE, D_MODEL, D_FF), D_MODEL ** -0.5),
        "dense_w_down": nrm(ks[12], (N_DENSE, D_FF, D_MODEL), D_FF ** -0.5),
        "router_w": nrm(ks[13], (N_MOE, D_MODEL, N_EXPERTS), D_MODEL ** -0.5),
        "router_b": nrm(ks[14], (N_MOE, N_EXPERTS), 0.01),
        "expert_w_gate": nrm(ks[15], (N_MOE, N_EXPERTS, D_MODEL, D_FF_EXPERT), D_MODEL ** -0.5),
        "expert_w_up": nrm(ks[16], (N_MOE, N_EXPERTS, D_MODEL, D_FF_EXPERT), D_MODEL ** -0.5),
        "expert_w_down": nrm(ks[17], (N_MOE, N_EXPERTS, D_FF_EXPERT, D_MODEL), D_FF_EXPERT ** -0.5),
        "final_norm_g": 1.0 + nrm(ks[18], (D_MODEL,), 0.02),
    }


def reference(x, mix_norm_g, w_in, sgu_norm_g, w_s, b_s, w_pa, w_pb, w_o, ffn_norm_g,
              dense_w_gate, dense_w_up, dense_w_down, router_w, router_b,
              expert_w_gate, expert_w_up, expert_w_down, final_norm_g):
    for i in range(DEPTH):
        h = rmsnorm(x, mix_norm_g[i])
        x = x + token_mixer(h, w_in[i], sgu_norm_g[i], w_s[i], b_s[i], w_pa[i], w_pb[i], w_o[i])
        h = rmsnorm(x, ffn_norm_g[i])
        j = i // 2
        if i % 2 == 0:
            x = x + swiglu(h, dense_w_gate[j], dense_w_up[j], dense_w_down[j])
        else:
            x = x + moe_swiglu(h, router_w[j], router_b[j], expert_w_gate[j], expert_w_up[j], expert_w_down[j])
    return rmsnorm(x, final_norm_g)
```

```python
import contextlib
import numpy as np
import concourse.bass as bass
import concourse.mybir as mybir
from concourse.bass_utils import run_bass_kernel_spmd

F32 = mybir.dt.float32
BF16 = mybir.dt.bfloat16
AF = mybir.ActivationFunctionType
ALU = mybir.AluOpType
AX = mybir.AxisListType

D = 2048
KC = 16
T = 1024
NT = 8
DFF = 5632
NFG = 11
NE = 8
EPS = 1e-6
BIG = 30000.0
IN_W = 9216
OFF_Q, OFF_K, OFF_V, OFF_U, OFF_VG, OFF_GA, OFF_GB = 0, 1024, 2048, 3072, 4096, 5120, 7168

ENGINES = ("pe", "act", "dve", "pool", "sp")
DMA_RING = 6


class Op:
    __slots__ = ("eng", "fn", "deps", "signal", "count", "is_dma", "slot", "target", "idx")


class Prog:
    def __init__(self, nc, st):
        self.nc = nc
        self.ops = []
        self.last_w = {}
        self.readers = {}
        self.dma_count = {e: 0 for e in ENGINES}
        self.dma_ops = {e: [] for e in ENGINES}
        self.emitted = 0
        self.cnt = {e: 0 for e in ENGINES}
        self.waited = {e: {} for e in ENGINES}
        self.csem = {e: st.enter_context(nc.semaphore("c_" + e)) for e in ENGINES}
        self.dsem = {e: [st.enter_context(nc.semaphore("d_%s_%d" % (e, i))) for i in range(DMA_RING)]
                     for e in ("sp", "pool")}
        self.nblocks = 0

    def op(self, eng, fn, reads=(), writes=(), dma=False):
        o = Op()
        o.eng, o.fn, o.is_dma = eng, fn, dma
        o.idx = len(self.ops)
        o.signal = dma
        o.count = None
        deps = set()
        for r in reads:
            for w in self.last_w.get(r, ()):
                deps.add(w)
        for r in writes:
            for w in self.last_w.get(r, ()):
                if dma and self.ops[w].is_dma:
                    continue
                deps.add(w)
            rd = self.readers.get(r)
            if rd:
                deps.update(rd[0].values())
                deps.update(rd[1])
        if dma:
            j = self.dma_count[eng]
            o.slot = j % DMA_RING
            o.target = 16 * (j // DMA_RING + 1)
            self.dma_count[eng] += 1
            lst = self.dma_ops[eng]
            if j >= DMA_RING:
                deps.add(lst[j - DMA_RING])
            lst.append(o.idx)
        deps.discard(o.idx)
        fdeps = []
        for d in deps:
            do = self.ops[d]
            if not do.is_dma:
                if d < self.emitted:
                    continue
                if do.eng == "pe" and eng == "pe" and not dma:
                    continue
                do.signal = True
            fdeps.append(d)
        o.deps = fdeps
        for r in reads:
            rd = self.readers.setdefault(r, ({}, []))
            if dma:
                rd[1].append(o.idx)
            else:
                rd[0][eng] = o.idx
        for r in writes:
            prev = self.last_w.get(r)
            rd = self.readers.get(r, ({}, []))
            if dma and prev and all(self.ops[w].is_dma for w in prev) and not rd[0] and not rd[1]:
                self.last_w[r] = prev + [o.idx]
            else:
                self.last_w[r] = [o.idx]
            self.readers[r] = ({}, [])
        self.ops.append(o)
        return o

    def emit_block(self, drain=("sp",), final=False):
        nc = self.nc
        new = self.ops[self.emitted:]
        for o in new:
            if not o.is_dma and o.signal:
                self.cnt[o.eng] += 1
                o.count = self.cnt[o.eng]
        dq = ("sp", "pool") if final else drain

        def run(engname):
            def body(eng):
                waited = self.waited[engname]

                def wait_for(do):
                    if do.is_dma:
                        key = ("d", do.eng, do.slot)
                        sem = self.dsem[do.eng][do.slot]
                        val = do.target
                    else:
                        key = ("c", do.eng)
                        sem = self.csem[do.eng]
                        val = do.count
                    if waited.get(key, 0) >= val:
                        return
                    waited[key] = val
                    eng.wait_ge(sem, val)

                for o in new:
                    if o.eng != engname:
                        continue
                    for d in sorted(o.deps):
                        wait_for(self.ops[d])
                    ins = o.fn(eng)
                    if o.is_dma:
                        ins.then_inc(self.dsem[o.eng][o.slot], 16)
                    elif o.signal:
                        ins.then_inc(self.csem[o.eng], 1)
                if engname in dq:
                    for oi in self.dma_ops[engname][-DMA_RING:]:
                        wait_for(self.ops[oi])
                if final and engname == "sp":
                    for oi in self.dma_ops["pool"][-DMA_RING:]:
                        wait_for(self.ops[oi])
            return body

        with nc.Block() as block:
            block.tensor(run("pe"))
            block.scalar(run("act"))
            block.vector(run("dve"))
            block.gpsimd(run("pool"))
            block.sync(run("sp"))
        self.emitted = len(self.ops)
        self.nblocks += 1


class WStream:
    NSLOT = 3
    LOOK = 1

    def __init__(self, P, nc, st):
        self.P = P
        self.slots = [st.enter_context(nc.sbuf_tensor("wslab%d" % i, [128, 8192], BF16)) for i in range(self.NSLOT)]
        self.plan = []
        self.next_load = 0
        self.next_get = 0

    def add(self, tag, a, b, src):
        self.plan.append((tag, a, b, src))

    def _view(self, i):
        tag, a, b, src = self.plan[i]
        t = self.slots[i % self.NSLOT]
        return t[:, 0:a * b].rearrange("p (a b) -> p a b", a=a)

    def get(self, tag):
        i = self.next_get
        assert self.plan[i][0] == tag, (self.plan[i][0], tag)
        lim = min(len(self.plan), i + self.LOOK + 1)
        while self.next_load < lim:
            j = self.next_load
            _, a, b, src = self.plan[j]
            dst = self._view(j)
            self.P.op("pool", lambda e, dst=dst, src=src: e.dma_start(out=dst, in_=src),
                      writes=[("slab", j % self.NSLOT)], dma=True)
            self.next_load += 1
        self.next_get += 1
        return self._view(i), ("slab", i % self.NSLOT)


class Ring:
    def __init__(self, items):
        self.items = items
        self.i = 0

    def next(self):
        it = self.items[self.i % len(self.items)]
        self.i += 1
        return it


class Ctx:
    pass


_UID = [0]


def _sb(st, nc, name, shape, dt):
    _UID[0] += 1
    return st.enter_context(nc.sbuf_tensor("%s_u%d" % (name, _UID[0]), shape, dt))


def _ps(st, nc, name, dt=F32):
    n = 512 if dt == F32 else 1024
    _UID[0] += 1
    return st.enter_context(nc.psum_tensor("%s_u%d" % (name, _UID[0]), [128, n], dt))


def colslab(w, c0):
    return w.rearrange("(kc p) n -> p kc n", p=128)[:, :, c0:c0 + 512]


def rowslab(w, r0):
    return w[r0:r0 + 512, :].rearrange("(kc p) n -> p kc n", p=128)


def ph_load(c):
    P, nc = c.P, c.nc
    for tt in range(NT):
        P.op("sp", lambda e, tt=tt: e.dma_start(out=c.X[:, tt, :], in_=c.d["x_own"][tt * 128:(tt + 1) * 128, :]),
             writes=[("X", tt)], dma=True)
    for name, t in (("ident", c.ident), ("ones", c.ones), ("esel", c.esel), ("causal", c.causal)):
        P.op("pool", lambda e, name=name, t=t: e.dma_start(out=t[:], in_=c.d[name][:, :]), writes=[name], dma=True)
    for name, t in (("gbias", c.gbias), ("valid", c.valid), ("ownm", c.ownm), ("tril", c.tril)):
        P.op("sp", lambda e, name=name, t=t: e.dma_start(out=t[:], in_=c.d[name][:, :]), writes=[name], dma=True)
    P.emit_block()


def ph_norm(c, gname, src_dram=None, final_out=None):
    P, nc = c.P, c.nc
    with contextlib.ExitStack() as st:
        gbc = _sb(st, nc, "n_gbc", [128, D], F32)
        junk = _sb(st, nc, "n_junk", [128, D], BF16)
        ss = _sb(st, nc, "n_ss", [128, NT], F32)
        rstd = _sb(st, nc, "n_rstd", [128, NT], F32)
        if final_out is None:
            xs = [_sb(st, nc, "n_xs%d" % i, [128, D], BF16) for i in range(2)]
            pT = [_ps(st, nc, "n_pT%d" % i, BF16) for i in range(2)]
        else:
            yo = [_sb(st, nc, "n_yo%d" % i, [128, D], F32) for i in range(2)]
        if src_dram is not None:
            xin = [_sb(st, nc, "n_xin%d" % i, [128, D], F32) for i in range(2)]
        P.op("sp", lambda e: e.dma_start(out=gbc[:], in_=c.d[gname][:, :]), writes=["n_gbc"], dma=True)
        ptc = 0
        for tt in range(NT):
            b = tt % 2
            if src_dram is not None:
                src = xin[b][:]
                srck = ("n_xin", b)
                P.op("sp", lambda e, b=b, tt=tt: e.dma_start(out=xin[b][:], in_=src_dram[tt * 128:(tt + 1) * 128, :]),
                     writes=[srck], dma=True)
            else:
                src = c.X[:, tt, :]
                srck = ("X", tt)
            P.op("act", lambda e, src=src, tt=tt: e.activation(out=junk[:], in_=src, func=AF.Square, accum_out=ss[:, tt:tt + 1]),
                 reads=[srck], writes=["n_junk", ("n_ss", tt)])
            P.op("dve", lambda e, tt=tt: e.tensor_scalar(out=rstd[:, tt:tt + 1], in0=ss[:, tt:tt + 1], scalar1=1.0 / D, scalar2=EPS,
                                                          op0=ALU.mult, op1=ALU.add), reads=[("n_ss", tt)], writes=[("n_rstd", tt)])
            P.op("act", lambda e, tt=tt: e.activation(out=rstd[:, tt:tt + 1], in_=rstd[:, tt:tt + 1], func=AF.Sqrt),
                 reads=[("n_rstd", tt)], writes=[("n_rstd", tt)])
            P.op("dve", lambda e, tt=tt: e.reciprocal(out=rstd[:, tt:tt + 1], in_=rstd[:, tt:tt + 1]),
                 reads=[("n_rstd", tt)], writes=[("n_rstd", tt)])
            if final_out is not None:
                P.op("dve", lambda e, src=src, tt=tt, b=b: e.scalar_tensor_tensor(out=yo[b][:], in0=src, scalar=rstd[:, tt:tt + 1], in1=gbc[:],
                                                                                    op0=ALU.mult, op1=ALU.mult),
                     reads=[srck, ("n_rstd", tt), "n_gbc"], writes=[("n_yo", b)])
                P.op("sp", lambda e, tt=tt, b=b: e.dma_start(out=final_out[tt * 128:(tt + 1) * 128, :], in_=yo[b][:]),
                     reads=[("n_yo", b)], dma=True)
                continue
            P.op("dve", lambda e, src=src, tt=tt, b=b: e.scalar_tensor_tensor(out=xs[b][:], in0=src, scalar=rstd[:, tt:tt + 1], in1=gbc[:],
                                                                                op0=ALU.mult, op1=ALU.mult),
                 reads=[srck, ("n_rstd", tt), "n_gbc"], writes=[("n_xs", b)])
            for half in range(2):
                pb = ptc % 2
                ptc += 1
                for j in range(8):
                    kc = half * 8 + j
                    P.op("pe", lambda e, b=b, kc=kc, pb=pb, j=j: e.transpose(out=pT[pb][:, j * 128:(j + 1) * 128],
                                                                           in_=xs[b][:, kc * 128:(kc + 1) * 128], identity=c.ident[:]),
                         reads=[("n_xs", b), "ident"], writes=[("n_pT", pb)])
                dst = c.HT[:, half * 8:(half + 1) * 8, tt * 128:(tt + 1) * 128]
                srcp = pT[pb][:, :].rearrange("p (a b) -> p a b", a=8)
                if half == 0:
                    P.op("act", lambda e, dst=dst, srcp=srcp: e.activation(out=dst, in_=srcp, func=AF.Copy),
                         reads=[("n_pT", pb)], writes=[("HT", tt)])
                else:
                    P.op("dve", lambda e, dst=dst, srcp=srcp: e.tensor_copy(out=dst, in_=srcp),
                         reads=[("n_pT", pb)], writes=[("HT", tt)])
        P.emit_block()


def plan_kv(c, li, side):
    w = c.d["w_in"]
    for s in range(2):
        c.WS.add(("k", li, side, s), KC, 512, colslab(w, OFF_K + s * 512))
    for s in range(2):
        c.WS.add(("v", li, side, s), KC, 512, colslab(w, OFF_V + s * 512))


def ph_kv(c, li, side):
    P, nc = c.P, c.nc
    HTALL = [("HT", tt) for tt in range(NT)]
    with contextlib.ExitStack() as st:
        kst = _sb(st, nc, "kv_kst", [128, 8, T], BF16)
        vst = _sb(st, nc, "kv_vst", [128, NT, 1024], BF16)
        ring = Ring([(_ps(st, nc, "kv_ps%d" % i), ("kv_ps", i)) for i in range(4)])
        for s in range(2):
            slab, sk = c.WS.get(("k", li, side, s))
            for hi in range(4):
                h = s * 4 + hi
                for tr in range(2):
                    ps, pk = ring.next()
                    for kc in range(KC):
                        P.op("pe", lambda e, ps=ps, slab=slab, kc=kc, hi=hi, tr=tr: e.matmul(
                            out=ps[:], lhsT=slab[:, kc, hi * 128:(hi + 1) * 128], rhs=c.HT[:, kc, tr * 512:(tr + 1) * 512],
                            start=(kc == 0), stop=(kc == KC - 1)), reads=[sk] + HTALL[tr * 4:(tr + 1) * 4], writes=[pk])
                    P.op("act", lambda e, ps=ps, h=h, tr=tr: e.activation(out=kst[:, h, tr * 512:(tr + 1) * 512], in_=ps[:], func=AF.Copy),
                         reads=[pk], writes=[("kst", h)])
        for s in range(2):
            slab, sk = c.WS.get(("v", li, side, s))
            for tt in range(NT):
                ps, pk = ring.next()
                for kc in range(KC):
                    P.op("pe", lambda e, ps=ps, slab=slab, kc=kc, tt=tt: e.matmul(
                        out=ps[:], lhsT=c.HT[:, kc, tt * 128:(tt + 1) * 128], rhs=slab[:, kc, :],
                        start=(kc == 0), stop=(kc == KC - 1)), reads=[sk, ("HT", tt)], writes=[pk])
                P.op("dve", lambda e, ps=ps, tt=tt, s=s: e.tensor_copy(out=vst[:, tt, s * 512:(s + 1) * 512], in_=ps[:]),
                     reads=[pk], writes=[("vst", s)])
        for h in range(8):
            P.op("sp", lambda e, h=h: e.dma_start(out=c.KT[side, h], in_=kst[:, h, :]),
                 reads=[("kst", h)], writes=[("KT", side, h)], dma=True)
            P.op("sp", lambda e, h=h: e.dma_start(out=c.VS[side, h].rearrange("(tt p) d -> p tt d", p=128),
                                                  in_=vst[:, :, h * 128:(h + 1) * 128]),
                 reads=[("vst", h // 4)], writes=[("VS", side, h)], dma=True)
        P.emit_block()


def plan_attn(c, li):
    for s in range(2):
        c.WS.add(("q", li, s), KC, 512, colslab(c.d["w_in"], OFF_Q + s * 512))


def ph_attn(c, li):
    P, nc = c.P, c.nc
    HTALL = [("HT", tt) for tt in range(NT)]
    scale = 128 ** -0.5
    with contextlib.ExitStack() as st:
        qT = [_sb(st, nc, "a_qT%d" % i, [128, T], BF16) for i in range(2)]
        kT = [_sb(st, nc, "a_kT%d" % i, [128, 2 * T], BF16) for i in range(1)] * 2
        Vh = [_sb(st, nc, "a_V%d" % i, [128, 16, 128], BF16) for i in range(1)] * 2
        kmb = [_sb(st, nc, "a_kmb%d" % i, [128, 8], BF16) for i in range(2)]
        km32 = _sb(st, nc, "a_km32", [128, 8], F32)
        gm = _sb(st, nc, "a_gm", [128, 64], F32)
        mx8 = _sb(st, nc, "a_mx8", [128, 64], F32)
        sel = _sb(st, nc, "a_sel", [128, 64], F32)
        biasb = _sb(st, nc, "a_biasb", [128, 64], BF16)
        biasT = [_sb(st, nc, "a_biasT%d" % i, [8, T], BF16) for i in range(1)] * 2
        expT = [_sb(st, nc, "a_expT%d" % i, [128, 512], BF16) for i in range(2)]
        rden = [_sb(st, nc, "a_rden%d" % i, [128, 512], F32) for i in range(1)] * 2
        S = [_ps(st, nc, "a_S%d" % i) for i in range(2)]
        O = _ps(st, nc, "a_O")
        Dn = _ps(st, nc, "a_D")
        Q = [_ps(st, nc, "a_Q%d" % i) for i in range(2)]
        PG = _ps(st, nc, "a_PG")
        PBT = _ps(st, nc, "a_PBT", BF16)
        qslab = None
        ec = 0
        rc = 0
        for h in range(8):
            hb = h % 2
            if h % 4 == 0:
                qslab, qk = c.WS.get(("q", li, h // 4))
            hi = h % 4
            for side in range(2):
                P.op("sp", lambda e, hb=hb, side=side, h=h: e.dma_start(out=kT[hb][:, side * T:(side + 1) * T], in_=c.KT[side, h]),
                     reads=[("KT", side, h)], writes=[("a_kT", 0)], dma=True)
                P.op("sp", lambda e, hb=hb, side=side, h=h: e.dma_start(
                    out=Vh[hb][:, side * 8:(side + 1) * 8, :], in_=c.VS[side, h].rearrange("(tt p) d -> p tt d", p=128)),
                    reads=[("VS", side, h)], writes=[("a_V", 0)], dma=True)
            for tr in range(2):
                for kc in range(KC):
                    P.op("pe", lambda e, tr=tr, kc=kc, hi=hi, qslab=qslab: e.matmul(
                        out=Q[tr][:], lhsT=qslab[:, kc, hi * 128:(hi + 1) * 128], rhs=c.HT[:, kc, tr * 512:(tr + 1) * 512],
                        start=(kc == 0), stop=(kc == KC - 1)), reads=[qk] + HTALL[tr * 4:(tr + 1) * 4], writes=[("a_Q", tr)])
                P.op("act", lambda e, tr=tr, hb=hb: e.activation(out=qT[hb][:, tr * 512:(tr + 1) * 512], in_=Q[tr][:], func=AF.Copy),
                     reads=[("a_Q", tr)], writes=[("a_qT", hb)])
            P.op("dve", lambda e, hb=hb: e.tensor_reduce(out=km32[:], in_=kT[hb][:, :].rearrange("p (n j) -> p n j", j=256),
                                                         axis=AX.X, op=ALU.add), reads=[("a_kT", 0)], writes=["a_km32"])
            P.op("dve", lambda e, hb=hb: e.tensor_copy(out=kmb[hb][:], in_=km32[:]), reads=["a_km32"], writes=[("a_kmb", hb)])
            for qt in range(NT):
                P.op("pe", lambda e, qt=qt, hb=hb: e.matmul(out=PG[:, qt * 8:(qt + 1) * 8], lhsT=qT[hb][:, qt * 128:(qt + 1) * 128],
                                                          rhs=kmb[hb][:], start=True, stop=True),
                     reads=[("a_qT", hb), ("a_kmb", hb)], writes=["a_PG"])
            P.op("dve", lambda e: e.tensor_tensor(out=gm[:], in0=PG[:, 0:64], in1=c.gbias[:], op=ALU.add),
                 reads=["a_PG", "gbias"], writes=["a_gm"])
            for qt in range(NT):
                P.op("dve", lambda e, qt=qt: e.max(out=mx8[:, qt * 8:(qt + 1) * 8], in_=gm[:, qt * 8:(qt + 1) * 8]),
                     reads=["a_gm"], writes=[("a_mx8", qt)])
            for qt in range(NT):
                P.op("dve", lambda e, qt=qt: e.tensor_scalar(out=sel[:, qt * 8:(qt + 1) * 8], in0=gm[:, qt * 8:(qt + 1) * 8],
                                                             scalar1=mx8[:, qt * 8 + 2:qt * 8 + 3], scalar2=None, op0=ALU.is_ge),
                     reads=["a_gm", ("a_mx8", qt)], writes=[("a_sel", qt)])
            SELALL = [("a_sel", qt) for qt in range(NT)]
            P.op("dve", lambda e: e.tensor_tensor(out=sel[:], in0=sel[:], in1=c.valid[:], op=ALU.mult),
                 reads=SELALL + ["valid"], writes=SELALL)
            P.op("dve", lambda e: e.tensor_tensor(out=sel[:], in0=sel[:], in1=c.ownm[:], op=ALU.add),
                 reads=SELALL + ["ownm"], writes=SELALL)
            P.op("dve", lambda e: e.tensor_scalar(out=biasb[:], in0=sel[:], scalar1=BIG, scalar2=-BIG, op0=ALU.mult, op1=ALU.add),
                 reads=SELALL, writes=["a_biasb"])
            for qt in range(NT):
                P.op("pe", lambda e, qt=qt: e.transpose(out=PBT[0:8, qt * 128:(qt + 1) * 128], in_=biasb[:, qt * 8:(qt + 1) * 8],
                                                        identity=c.ident[:]), reads=["a_biasb", "ident"], writes=["a_PBT"])
            P.op("dve", lambda e, hb=hb: e.tensor_copy(out=biasT[hb][:], in_=PBT[0:8, :]), reads=["a_PBT"], writes=[("a_biasT", 0)])
            for qr in range(2):
                nkt = 8 + 4 * (qr + 1)

                def emit_S(kt, qr=qr, hb=hb):
                    sb = kt % 2
                    n = kt // 2
                    P.op("pe", lambda e: e.matmul(out=S[sb][:], lhsT=kT[hb][:, kt * 128:(kt + 1) * 128],
                                                  rhs=qT[hb][:, qr * 512:(qr + 1) * 512], start=True, stop=False),
                         reads=[("a_kT", 0), ("a_qT", hb)], writes=[("a_S", sb)])
                    P.op("pe", lambda e: e.matmul(out=S[sb][:], lhsT=c.esel[0:8, n * 128:(n + 1) * 128],
                                                  rhs=biasT[hb][0:8, qr * 512:(qr + 1) * 512], start=False, stop=True),
                         reads=["esel", ("a_biasT", 0)], writes=[("a_S", sb)])

                emit_S(0)
                for kt in range(nkt):
                    if kt + 1 < nkt:
                        emit_S(kt + 1)
                    sb = kt % 2
                    eb = ec % 2
                    ec += 1
                    P.op("act", lambda e, sb=sb, eb=eb: e.activation(out=expT[eb][:], in_=S[sb][:], func=AF.Exp, scale=scale),
                         reads=[("a_S", sb)], writes=[("a_expT", eb)])
                    n = kt // 2
                    r = kt % 2
                    A = 4 + 2 * qr
                    if n == A or n == A + 1:
                        c0 = 0 if n == A else 256
                        P.op("dve", lambda e, eb=eb, c0=c0, r=r: e.tensor_tensor(out=expT[eb][:, c0:c0 + 256], in0=expT[eb][:, c0:c0 + 256],
                                                                               in1=c.causal[:, r * 256:(r + 1) * 256], op=ALU.mult),
                             reads=[("a_expT", eb), "causal"], writes=[("a_expT", eb)])
                    P.op("pe", lambda e, eb=eb, kt=kt, hb=hb, nkt=nkt: e.matmul(out=O[:], lhsT=Vh[hb][:, kt, :], rhs=expT[eb][:],
                                                                              start=(kt == 0), stop=(kt == nkt - 1)),
                         reads=[("a_V", 0), ("a_expT", eb)], writes=["a_O"])
                    P.op("pe", lambda e, eb=eb, kt=kt, nkt=nkt: e.matmul(out=Dn[:], lhsT=c.ones[:, :], rhs=expT[eb][:],
                                                                       start=(kt == 0), stop=(kt == nkt - 1)),
                         reads=["ones", ("a_expT", eb)], writes=["a_D"])
                rb = rc % 2
                rc += 1
                P.op("dve", lambda e, rb=rb: e.reciprocal(out=rden[rb][:], in_=Dn[:]), reads=["a_D"], writes=[("a_rden", 0)])
                P.op("dve", lambda e, rb=rb, h=h, qr=qr: e.tensor_tensor(out=c.AT[:, h, qr * 512:(qr + 1) * 512], in0=O[:], in1=rden[rb][:], op=ALU.mult),
                     reads=["a_O", ("a_rden", 0)], writes=[("AT", h)])
        P.emit_block()


def plan_sgu(c, li):
    for s in range(2):
        c.WS.add(("vg", li, s), KC, 512, colslab(c.d["w_in"], OFF_VG + s * 512))
    for s in range(2):
        c.WS.add(("u", li, s), KC, 512, colslab(c.d["w_in"], OFF_U + s * 512))


def ph_sgu(c, li):
    P, nc = c.P, c.nc
    HTALL = [("HT", tt) for tt in range(NT)]
    with contextlib.ExitStack() as st:
        gv = _sb(st, nc, "s_gv", [128, NT, 1024], BF16)
        s1 = _sb(st, nc, "s_s1", [128, NT, 2], F32)
        s2 = _sb(st, nc, "s_s2", [128, NT], F32)
        mean = _sb(st, nc, "s_mean", [128, NT], F32)
        var = _sb(st, nc, "s_var", [128, NT], F32)
        msq = _sb(st, nc, "s_msq", [128, NT], F32)
        junk = _sb(st, nc, "s_junk", [128, 1024], BF16)
        gsg = _sb(st, nc, "s_gsg", [128, 1024], F32)
        ws32 = _sb(st, nc, "s_ws32", [128, 8, 128], F32)
        wsb = _sb(st, nc, "s_wsb", [128, 8, 128], BF16)
        bsb = _sb(st, nc, "s_bsb", [1, 1024], BF16)
        uT = [_sb(st, nc, "s_uT%d" % i, [128, T], BF16) for i in range(2)]
        tmp = [_sb(st, nc, "s_tmp%d" % i, [128, 1024], F32) for i in range(1)] * 2
        ring = Ring([(_ps(st, nc, "s_ps%d" % i), ("s_ps", i)) for i in range(4)])
        pmr = Ring([(_ps(st, nc, "s_pm%d" % i), ("s_pm", i)) for i in range(2)])
        P.op("sp", lambda e: e.dma_start(out=gsg[:], in_=c.d["g_sgu"][:, :]), writes=["s_gsg"], dma=True)
        P.op("sp", lambda e: e.dma_start(out=ws32[:], in_=c.d["wsT"][:, :, :]), writes=["s_ws32"], dma=True)
        P.op("pool", lambda e: e.dma_start(out=bsb[:], in_=c.d["b_s"][:, :]), writes=["s_bsb"], dma=True)
        for g in range(8):
            P.op("dve", lambda e, g=g: e.tensor_tensor(out=wsb[:, g, :], in0=ws32[:, g, :], in1=c.tril[:], op=ALU.mult),
                 reads=["s_ws32", "tril"], writes=[("s_wsb", g)])
        for cr in range(2):
            slab, sk = c.WS.get(("vg", li, cr))
            for tt in range(NT):
                ps, pk = ring.next()
                for kc in range(KC):
                    P.op("pe", lambda e, ps=ps, slab=slab, kc=kc, tt=tt: e.matmul(
                        out=ps[:], lhsT=c.HT[:, kc, tt * 128:(tt + 1) * 128], rhs=slab[:, kc, :],
                        start=(kc == 0), stop=(kc == KC - 1)), reads=[sk, ("HT", tt)], writes=[pk])
                P.op("act", lambda e, ps=ps, tt=tt, cr=cr: e.activation(out=gv[:, tt, cr * 512:(cr + 1) * 512], in_=ps[:], func=AF.Gelu,
                                                                       accum_out=s1[:, tt, cr:cr + 1]),
                     reads=[pk], writes=[("s_gv", tt, cr), ("s_s1", tt, cr)])
        for tt in range(NT):
            P.op("act", lambda e, tt=tt: e.activation(out=junk[:], in_=gv[:, tt, :], func=AF.Square, accum_out=s2[:, tt:tt + 1]),
                 reads=[("s_gv", tt, 0), ("s_gv", tt, 1)], writes=["s_junk", ("s_s2", tt)])
        S1ALL = [("s_s1", tt, cr) for tt in range(NT) for cr in range(2)]
        S2ALL = [("s_s2", tt) for tt in range(NT)]
        P.op("dve", lambda e: e.tensor_tensor(out=mean[:], in0=s1[:, :, 0], in1=s1[:, :, 1], op=ALU.add), reads=S1ALL, writes=["s_mean"])
        P.op("dve", lambda e: e.tensor_scalar(out=mean[:], in0=mean[:], scalar1=1.0 / 1024, scalar2=None, op0=ALU.mult),
             reads=["s_mean"], writes=["s_mean"])
        P.op("dve", lambda e: e.tensor_tensor(out=msq[:], in0=mean[:], in1=mean[:], op=ALU.mult), reads=["s_mean"], writes=["s_msq"])
        P.op("dve", lambda e: e.scalar_tensor_tensor(out=var[:], in0=s2[:], scalar=1.0 / 1024, in1=msq[:], op0=ALU.mult, op1=ALU.subtract),
             reads=S2ALL + ["s_msq"], writes=["s_var"])
        P.op("dve", lambda e: e.tensor_scalar(out=var[:], in0=var[:], scalar1=EPS, scalar2=None, op0=ALU.add), reads=["s_var"], writes=["s_var"])
        P.op("act", lambda e: e.activation(out=var[:], in_=var[:], func=AF.Sqrt), reads=["s_var"], writes=["s_var"])
        P.op("dve", lambda e: e.reciprocal(out=var[:], in_=var[:]), reads=["s_var"], writes=["s_var"])
        for tt in range(NT):
            tb = tt % 2
            P.op("dve", lambda e, tt=tt, tb=tb: e.tensor_scalar(out=tmp[tb][:], in0=gv[:, tt, :], scalar1=mean[:, tt:tt + 1], scalar2=var[:, tt:tt + 1],
                                                               op0=ALU.subtract, op1=ALU.mult),
                 reads=[("s_gv", tt, 0), ("s_gv", tt, 1), "s_mean", "s_var"], writes=[("s_tmp", 0)])
            P.op("dve", lambda e, tt=tt, tb=tb: e.tensor_tensor(out=gv[:, tt, :], in0=tmp[tb][:], in1=gsg[:], op=ALU.mult),
                 reads=[("s_tmp", 0), "s_gsg"], writes=[("s_gv", tt, 0), ("s_gv", tt, 1)])
        for us in range(2):
            slab, sk = c.WS.get(("u", li, us))
            for gi in range(4):
                g = us * 4 + gi
                ub = g % 2
                for tr in range(2):
                    ps, pk = ring.next()
                    for kc in range(KC):
                        P.op("pe", lambda e, ps=ps, slab=slab, kc=kc, gi=gi, tr=tr: e.matmul(
                            out=ps[:], lhsT=slab[:, kc, gi * 128:(gi + 1) * 128], rhs=c.HT[:, kc, tr * 512:(tr + 1) * 512],
                            start=(kc == 0), stop=(kc == KC - 1)), reads=[sk] + HTALL[tr * 4:(tr + 1) * 4], writes=[pk])
                    P.op("act", lambda e, ps=ps, ub=ub, tr=tr: e.activation(out=uT[ub][:, tr * 512:(tr + 1) * 512], in_=ps[:], func=AF.Gelu),
                         reads=[pk], writes=[("s_uT", ub, tr)])
                for half in range(2):
                    pm, pmk = pmr.next()
                    for j in range(4):
                        tt = half * 4 + j
                        P.op("pe", lambda e, pm=pm, j=j, tt=tt, g=g: e.matmul(out=pm[:, j * 128:(j + 1) * 128], lhsT=gv[:, tt, g * 128:(g + 1) * 128],
                                                                          rhs=wsb[:, g, :], start=True, stop=False),
                             reads=[("s_gv", tt, g // 4), ("s_wsb", g)], writes=[pmk])
                        P.op("pe", lambda e, pm=pm, j=j, g=g: e.matmul(out=pm[:, j * 128:(j + 1) * 128], lhsT=c.ones[0:1, 0:128],
                                                                   rhs=bsb[0:1, g * 128:(g + 1) * 128], start=False, stop=True),
                             reads=["ones", "s_bsb"], writes=[pmk])
                    P.op("dve", lambda e, pm=pm, ub=ub, g=g, half=half: e.tensor_tensor(
                        out=c.ST[:, g, half * 512:(half + 1) * 512], in0=pm[:], in1=uT[ub][:, half * 512:(half + 1) * 512], op=ALU.mult),
                        reads=[pmk, ("s_uT", ub, half)], writes=[("ST", g)])
        P.emit_block()


def plan_merge(c, li):
    for cg in range(4):
        c.WS.add(("ga", li, cg), KC, 512, colslab(c.d["w_in"], OFF_GA + cg * 512))
        c.WS.add(("pa", li, cg), 8, 512, colslab(c.d["w_pa"], cg * 512))
        c.WS.add(("gb", li, cg), KC, 512, colslab(c.d["w_in"], OFF_GB + cg * 512))
        c.WS.add(("pb", li, cg), 8, 512, colslab(c.d["w_pb"], cg * 512))
        c.WS.add(("wo", li, cg), 4, D, rowslab(c.d["w_o"], cg * 512))


def ph_merge(c, li):
    P, nc = c.P, c.nc
    HTALL = [("HT", tt) for tt in range(NT)]
    ATALL = [("AT", h) for h in range(8)]
    STALL = [("ST", g) for g in range(8)]
    with contextlib.ExitStack() as st:
        mg = [_sb(st, nc, "m_mg%d" % i, [128, 4, T], BF16) for i in range(2)]
        sg = [_sb(st, nc, "m_sg%d" % i, [128, 512], F32) for i in range(2)]
        m32 = [_sb(st, nc, "m_m32%d" % i, [128, 512], F32) for i in range(1)] * 2
        ringA = Ring([(_ps(st, nc, "m_pa%d" % i), ("m_pa", i)) for i in range(2)])
        ringG = Ring([(_ps(st, nc, "m_pg%d" % i), ("m_pg", i)) for i in range(2)])
        ringO = Ring([(_ps(st, nc, "m_po%d" % i), ("m_po", i)) for i in range(4)])
        sc = 0
        for cg in range(4):
            mb = cg % 2
            for br in range(2):
                gsl, gk = c.WS.get(("ga" if br == 0 else "gb", li, cg))
                psl, pk_ = c.WS.get(("pa" if br == 0 else "pb", li, cg))
                src = c.AT if br == 0 else c.ST
                srck = ATALL if br == 0 else STALL
                for ci in range(4):
                    for tr in range(2):
                        pG, pGk = ringG.next()
                        pA, pAk = ringA.next()
                        sb = sc % 2
                        sc += 1
                        for kc in range(KC):
                            P.op("pe", lambda e, pG=pG, gsl=gsl, kc=kc, ci=ci, tr=tr: e.matmul(
                                out=pG[:], lhsT=gsl[:, kc, ci * 128:(ci + 1) * 128], rhs=c.HT[:, kc, tr * 512:(tr + 1) * 512],
                                start=(kc == 0), stop=(kc == KC - 1)), reads=[gk] + HTALL[tr * 4:(tr + 1) * 4], writes=[pGk])
                        P.op("act", lambda e, pG=pG, sb=sb: e.activation(out=sg[sb][:], in_=pG[:], func=AF.Sigmoid),
                             reads=[pGk], writes=[("m_sg", sb)])
                        for kc in range(8):
                            P.op("pe", lambda e, pA=pA, psl=psl, kc=kc, ci=ci, tr=tr, src=src: e.matmul(
                                out=pA[:], lhsT=psl[:, kc, ci * 128:(ci + 1) * 128], rhs=src[:, kc, tr * 512:(tr + 1) * 512],
                                start=(kc == 0), stop=(kc == 7)), reads=[pk_] + srck, writes=[pAk])
                        if br == 0:
                            P.op("dve", lambda e, pA=pA, sb=sb, mb=mb, ci=ci, tr=tr: e.tensor_tensor(
                                out=mg[mb][:, ci, tr * 512:(tr + 1) * 512], in0=pA[:], in1=sg[sb][:], op=ALU.mult),
                                reads=[pAk, ("m_sg", sb)], writes=[("m_mg", mb, ci, tr)])
                        else:
                            P.op("dve", lambda e, pA=pA, sb=sb: e.tensor_tensor(out=m32[sb][:], in0=pA[:], in1=sg[sb][:], op=ALU.mult),
                                 reads=[pAk, ("m_sg", sb)], writes=[("m_m32", 0)])
                            P.op("dve", lambda e, sb=sb, mb=mb, ci=ci, tr=tr: e.tensor_tensor(
                                out=mg[mb][:, ci, tr * 512:(tr + 1) * 512], in0=mg[mb][:, ci, tr * 512:(tr + 1) * 512], in1=m32[sb][:], op=ALU.add),
                                reads=[("m_m32", 0), ("m_mg", mb, ci, tr)], writes=[("m_mg", mb, ci, tr)])
            wsl, wk = c.WS.get(("wo", li, cg))
            for tt in range(NT):
                tr = tt // 4
                for cr in range(4):
                    po, pok = ringO.next()
                    for ci in range(4):
                        P.op("pe", lambda e, po=po, wsl=wsl, ci=ci, tt=tt, cr=cr, mb=mb: e.matmul(
                            out=po[:], lhsT=mg[mb][:, ci, tt * 128:(tt + 1) * 128], rhs=wsl[:, ci, cr * 512:(cr + 1) * 512],
                            start=(ci == 0), stop=(ci == 3)), reads=[wk, ("m_mg", mb, ci, tr)], writes=[pok])
                    P.op("dve", lambda e, po=po, tt=tt, cr=cr: e.tensor_tensor(out=c.X[:, tt, cr * 512:(cr + 1) * 512],
                                                                             in0=c.X[:, tt, cr * 512:(cr + 1) * 512], in1=po[:], op=ALU.add),
                         reads=[pok, ("X", tt)], writes=[("X", tt)])
        P.emit_block()


def plan_ffn(c, li, wg, wu, wd, tagp):
    for fg in range(NFG):
        c.WS.add((tagp, "g", fg), KC, 512, colslab(wg, fg * 512))
        c.WS.add((tagp, "u", fg), KC, 512, colslab(wu, fg * 512))
        c.WS.add((tagp, "d", fg), 4, D, rowslab(wd, fg * 512))


def ffn_body(c, st, tagp, comb=None, e_idx=None):
    P, nc = c.P, c.nc
    HTALL = [("HT", tt) for tt in range(NT)]
    r = c.ffn_res
    for fg in range(NFG):
        ab = r["ac"] % 2
        r["ac"] += 1
        gsl, gk = c.WS.get((tagp, "g", fg))
        usl, uk = c.WS.get((tagp, "u", fg))
        for fi in range(4):
            for tr in range(2):
                pG, pGk = r["ringG"].next()
                pU, pUk = r["ringU"].next()
                sb = r["sc"] % 2
                r["sc"] += 1
                for kc in range(KC):
                    P.op("pe", lambda e, pG=pG, gsl=gsl, kc=kc, fi=fi, tr=tr: e.matmul(
                        out=pG[:], lhsT=gsl[:, kc, fi * 128:(fi + 1) * 128], rhs=c.HT[:, kc, tr * 512:(tr + 1) * 512],
                        start=(kc == 0), stop=(kc == KC - 1)), reads=[gk] + HTALL[tr * 4:(tr + 1) * 4], writes=[pGk])
                P.op("act", lambda e, pG=pG, sb=sb: e.activation(out=r["sg"][sb][:], in_=pG[:], func=AF.Silu),
                     reads=[pGk], writes=[("f_sg", sb)])
                for kc in range(KC):
                    P.op("pe", lambda e, pU=pU, usl=usl, kc=kc, fi=fi, tr=tr: e.matmul(
                        out=pU[:], lhsT=usl[:, kc, fi * 128:(fi + 1) * 128], rhs=c.HT[:, kc, tr * 512:(tr + 1) * 512],
                        start=(kc == 0), stop=(kc == KC - 1)), reads=[uk] + HTALL[tr * 4:(tr + 1) * 4], writes=[pUk])
                P.op("dve", lambda e, pU=pU, sb=sb, ab=ab, fi=fi, tr=tr: e.tensor_tensor(
                    out=r["act"][ab][:, fi, tr * 512:(tr + 1) * 512], in0=pU[:], in1=r["sg"][sb][:], op=ALU.mult),
                    reads=[pUk, ("f_sg", sb)], writes=[("f_act", ab, fi, tr)])
        dsl, dk = c.WS.get((tagp, "d", fg))
        for tt in range(NT):
            tr = tt // 4
            for cr in range(4):
                po, pok = r["ringO"].next()
                for fi in range(4):
                    P.op("pe", lambda e, po=po, dsl=dsl, fi=fi, tt=tt, cr=cr, ab=ab: e.matmul(
                        out=po[:], lhsT=r["act"][ab][:, fi, tt * 128:(tt + 1) * 128], rhs=dsl[:, fi, cr * 512:(cr + 1) * 512],
                        start=(fi == 0), stop=(fi == 3)), reads=[dk, ("f_act", ab, fi, tr)], writes=[pok])
                xs = c.X[:, tt, cr * 512:(cr + 1) * 512]
                if comb is None:
                    P.op("dve", lambda e, po=po, xs=xs: e.tensor_tensor(out=xs, in0=xs, in1=po[:], op=ALU.add),
                         reads=[pok, ("X", tt)], writes=[("X", tt)])
                else:
                    P.op("dve", lambda e, po=po, xs=xs, tt=tt: e.scalar_tensor_tensor(
                        out=xs, in0=po[:], scalar=comb[:, tt * NE + e_idx:tt * NE + e_idx + 1], in1=xs, op0=ALU.mult, op1=ALU.add),
                        reads=[pok, ("X", tt), "r_comb"], writes=[("X", tt)])


def ffn_alloc(c, st):
    nc = c.nc
    c.ffn_res = dict(
        act=[_sb(st, nc, "f_act%d" % i, [128, 4, T], BF16) for i in range(2)],
        sg=[_sb(st, nc, "f_sg%d" % i, [128, 512], F32) for i in range(2)],
        ringG=Ring([(_ps(st, nc, "f_pg%d" % i), ("f_pg", i)) for i in range(2)]),
        ringU=Ring([(_ps(st, nc, "f_pu%d" % i), ("f_pu", i)) for i in range(2)]),
        ringO=Ring([(_ps(st, nc, "f_po%d" % i), ("f_po", i)) for i in range(4)]),
        ac=0, sc=0)


def ph_ffn_dense(c, li):
    with contextlib.ExitStack() as st:
        ffn_alloc(c, st)
        ffn_body(c, st, "ffn")
        c.P.emit_block()


def ph_moe_dense(c, li):
    P, nc = c.P, c.nc
    with contextlib.ExitStack() as st:
        wr = _sb(st, nc, "r_wr", [128, KC * NE], BF16)
        rb = _sb(st, nc, "r_rb", [128, NE], F32)
        lg = _sb(st, nc, "r_lg", [128, NT * NE], F32)
        mx = _sb(st, nc, "r_mx", [128, NT * NE], F32)
        dd = _sb(st, nc, "r_dd", [128, NT], F32)
        w1 = _sb(st, nc, "r_w1", [128, NT], F32)
        w2 = _sb(st, nc, "r_w2", [128, NT], F32)
        c1 = _sb(st, nc, "r_c1", [128, NT * NE], F32)
        comb = _sb(st, nc, "r_comb", [128, NT * NE], F32)
        ffn_alloc(c, st)
        PR = c.ffn_res["ringO"].items[0]
        P.op("pool", lambda e: e.dma_start(out=wr[:], in_=c.d["router_w"][:, :]), writes=["r_wr"], dma=True)
        P.op("sp", lambda e: e.dma_start(out=rb[:], in_=c.d["router_b"][:, :]), writes=["r_rb"], dma=True)
        for tt in range(NT):
            for kc in range(KC):
                P.op("pe", lambda e, tt=tt, kc=kc: e.matmul(out=PR[0][:, tt * NE:(tt + 1) * NE], lhsT=c.HT[:, kc, tt * 128:(tt + 1) * 128],
                                                          rhs=wr[:, kc * NE:(kc + 1) * NE], start=(kc == 0), stop=(kc == KC - 1)),
                     reads=["r_wr", ("HT", tt)], writes=[PR[1]])
        for tt in range(NT):
            sl = slice(tt * NE, (tt + 1) * NE)
            P.op("dve", lambda e, sl=sl: e.tensor_tensor(out=lg[:, sl], in0=PR[0][:, sl], in1=rb[:], op=ALU.add),
                 reads=[PR[1], "r_rb"], writes=[("r_lg", tt)])
            P.op("dve", lambda e, sl=sl: e.max(out=mx[:, sl], in_=lg[:, sl]), reads=[("r_lg", tt)], writes=[("r_mx", tt)])
            P.op("dve", lambda e, tt=tt: e.tensor_tensor(out=dd[:, tt:tt + 1], in0=mx[:, tt * NE:tt * NE + 1], in1=mx[:, tt * NE + 1:tt * NE + 2],
                                                       op=ALU.subtract), reads=[("r_mx", tt)], writes=[("r_dd", tt)])
            P.op("act", lambda e, tt=tt: e.activation(out=w1[:, tt:tt + 1], in_=dd[:, tt:tt + 1], func=AF.Sigmoid),
                 reads=[("r_dd", tt)], writes=[("r_w1", tt)])
            P.op("act", lambda e, tt=tt: e.activation(out=w2[:, tt:tt + 1], in_=dd[:, tt:tt + 1], func=AF.Sigmoid, scale=-1.0),
                 reads=[("r_dd", tt)], writes=[("r_w2", tt)])
            P.op("dve", lambda e, sl=sl, tt=tt: e.tensor_scalar(out=c1[:, sl], in0=lg[:, sl], scalar1=mx[:, tt * NE:tt * NE + 1], scalar2=w1[:, tt:tt + 1],
                                                               op0=ALU.is_equal, op1=ALU.mult),
                 reads=[("r_lg", tt), ("r_mx", tt), ("r_w1", tt)], writes=[("r_c1", tt)])
            P.op("dve", lambda e, sl=sl, tt=tt: e.tensor_scalar(out=comb[:, sl], in0=lg[:, sl], scalar1=mx[:, tt * NE + 1:tt * NE + 2], scalar2=w2[:, tt:tt + 1],
                                                               op0=ALU.is_equal, op1=ALU.mult),
                 reads=[("r_lg", tt), ("r_mx", tt), ("r_w2", tt)], writes=[("r_c2", tt)])
            P.op("dve", lambda e, sl=sl: e.tensor_tensor(out=comb[:, sl], in0=comb[:, sl], in1=c1[:, sl], op=ALU.add),
                 reads=[("r_c1", tt), ("r_c2", tt)], writes=["r_comb"])
        if c.dbg is not None and "comb" in c.dbg:
            P.op("sp", lambda e: e.dma_start(out=c.dbg["comb"][:, :], in_=comb[:]), reads=["r_comb"], dma=True)
        for ex in range(NE):
            ffn_body(c, st, ("moe", ex), comb=comb, e_idx=ex)
            P.emit_block()


def ph_store_x(c, out):
    P = c.P
    for tt in range(NT):
        P.op("sp", lambda e, tt=tt: e.dma_start(out=out[tt * 128:(tt + 1) * 128, :], in_=c.X[:, tt, :]), reads=[("X", tt)], dma=True)
    P.emit_block()


def build(layers, final_norm, prev_from_input=True, dbg_names=()):
    nc = bass.Bass("TRN2", target_bir_lowering=False)
    c = Ctx()
    c.nc = nc
    d = {}

    def din(name, shape):
        d[name] = nc.dram_tensor(name, list(shape), F32, kind="ExternalInput").ap()

    din("x_own", [T, D])
    din("x_prev", [T, D])
    for name, shape in (("ident", [128, 128]), ("ones", [128, 128]), ("esel", [8, 1024]), ("causal", [128, 512]),
                        ("gbias", [128, 64]), ("valid", [128, 64]), ("ownm", [128, 64]), ("tril", [128, 128])):
        din(name, shape)
    for s, kind in layers:
        for name, shape in (("g_mix", [128, D]), ("w_in", [D, IN_W]), ("g_sgu", [128, 1024]), ("wsT", [128, 8, 128]), ("b_s", [1, 1024]),
                            ("w_pa", [1024, D]), ("w_pb", [1024, D]), ("w_o", [D, D]), ("g_ffn", [128, D])):
            din("%s_%d" % (name, s), shape)
        if kind == "dense":
            din("wg_%d" % s, [D, DFF])
            din("wu_%d" % s, [D, DFF])
            din("wd_%d" % s, [DFF, D])
        else:
            din("router_w_%d" % s, [128, KC * NE])
            din("router_b_%d" % s, [128, NE])
            din("ewg_%d" % s, [NE, D, DFF])
            din("ewu_%d" % s, [NE, D, DFF])
            din("ewd_%d" % s, [NE, DFF, D])
    if final_norm:
        din("g_fin", [128, D])
    y = nc.dram_tensor("y", [T, D], F32, kind="ExternalOutput").ap()
    c.dbg = {}
    for name, shape in dbg_names:
        c.dbg[name] = nc.dram_tensor("dbg_" + name, list(shape), F32, kind="ExternalOutput").ap()
    c.KT = nc.dram_tensor("kt_scr", [2, 8, 128, T], BF16, kind="Internal").ap()
    c.VS = nc.dram_tensor("vs_scr", [2, 8, T, 128], BF16, kind="Internal").ap()

    with contextlib.ExitStack() as st:
        c.P = Prog(nc, st)
        c.WS = WStream(c.P, nc, st)
        c.X = _sb(st, nc, "X", [128, NT, D], F32)
        c.HT = _sb(st, nc, "HT", [128, KC, T], BF16)
        c.ident = _sb(st, nc, "ident", [128, 128], BF16)
        c.ones = _sb(st, nc, "ones", [128, 128], BF16)
        c.esel = _sb(st, nc, "esel", [8, 1024], BF16)
        c.causal = _sb(st, nc, "causal", [128, 512], BF16)
        c.gbias = _sb(st, nc, "gbias", [128, 64], F32)
        c.valid = _sb(st, nc, "valid", [128, 64], F32)
        c.ownm = _sb(st, nc, "ownm", [128, 64], F32)
        c.tril = _sb(st, nc, "tril", [128, 128], F32)

        base = dict(d)
        for s, kind in layers:
            c.d = dict(base)
            for name in ("g_mix", "w_in", "g_sgu", "wsT", "b_s", "w_pa", "w_pb", "w_o", "g_ffn"):
                c.d[name] = d["%s_%d" % (name, s)]
            plan_kv(c, s, 0)
            plan_kv(c, s, 1)
            plan_sgu(c, s)
            plan_attn(c, s)
            plan_merge(c, s)
            if kind == "dense":
                plan_ffn(c, s, d["wg_%d" % s], d["wu_%d" % s], d["wd_%d" % s], "ffn")
            else:
                for ex in range(NE):
                    plan_ffn(c, s, d["ewg_%d" % s][ex], d["ewu_%d" % s][ex], d["ewd_%d" % s][ex], ("moe", ex))

        c.d = dict(base)
        ph_load(c)
        for s, kind in layers:
            c.d = dict(base)
            for name in ("g_mix", "w_in", "g_sgu", "wsT", "b_s", "w_pa", "w_pb", "w_o", "g_ffn"):
                c.d[name] = d["%s_%d" % (name, s)]
            if kind != "dense":
                c.d["router_w"] = d["router_w_%d" % s]
                c.d["router_b"] = d["router_b_%d" % s]
            ph_norm(c, "g_mix", src_dram=d["x_prev"])
            ph_kv(c, s, 0)
            ph_norm(c, "g_mix")
            ph_kv(c, s, 1)
            with contextlib.ExitStack() as mst:
                c.ST = _sb(mst, nc, "ST", [128, 8, T], BF16)
                ph_sgu(c, s)
                c.AT = _sb(mst, nc, "AT", [128, 8, T], BF16)
                ph_attn(c, s)
                ph_merge(c, s)
            ph_norm(c, "g_ffn")
            if kind == "dense":
                ph_ffn_dense(c, s)
            else:
                ph_moe_dense(c, s)
        if final_norm:
            c.d = dict(base)
            ph_norm(c, "g_fin", final_out=y)
        else:
            ph_store_x(c, y)
        c.P.emit_block(final=True)
    return nc


def _consts(half):
    ident = np.eye(128, dtype=np.float32)
    ones = np.ones((128, 128), np.float32)
    esel = np.zeros((8, 8, 128), np.float32)
    for n in range(8):
        esel[n, n, :] = 1.0
    causal = np.zeros((128, 2, 256), np.float32)
    j = np.arange(128)[:, None]
    i = np.arange(256)[None, :]
    for r in range(2):
        causal[:, r, :] = (128 * r + j <= i)
    tril = (np.arange(128)[None, :] >= np.arange(128)[:, None]).astype(np.float32)
    gbias = np.zeros((8, 8), np.float32)
    valid = np.zeros((8, 8), np.float32)
    ownm = np.zeros((8, 8), np.float32)
    first = 0 if half == 1 else 4
    for qt in range(8):
        lb = 4 + qt // 2
        for n in range(8):
            ok = (n < lb) and (n >= first)
            valid[qt, n] = 1.0 if ok else 0.0
            gbias[qt, n] = 0.0 if ok else -1e30
            ownm[qt, n] = 1.0 if n == lb else 0.0
    rep = lambda a: np.ascontiguousarray(np.broadcast_to(a.reshape(1, 64), (128, 64)))
    return dict(ident=ident, ones=ones, esel=np.ascontiguousarray(esel.reshape(8, 1024)), causal=np.ascontiguousarray(causal.reshape(128, 512)),
                tril=tril, gbias=rep(gbias), valid=rep(valid), ownm=rep(ownm))


def _bc(v):
    v = np.asarray(v, np.float32).reshape(1, -1)
    return np.ascontiguousarray(np.broadcast_to(v, (128, v.shape[1])))


def _layer_inputs(inp, i, slot, kind):
    m = {}
    m["g_mix_%d" % slot] = _bc(inp["mix_norm_g"][i])
    m["w_in_%d" % slot] = np.ascontiguousarray(inp["w_in"][i])
    m["g_sgu_%d" % slot] = _bc(inp["sgu_norm_g"][i])
    m["wsT_%d" % slot] = np.ascontiguousarray(np.transpose(inp["w_s"][i], (2, 0, 1)))
    m["b_s_%d" % slot] = np.ascontiguousarray(inp["b_s"][i].reshape(1, 1024))
    m["w_pa_%d" % slot] = np.ascontiguousarray(inp["w_pa"][i])
    m["w_pb_%d" % slot] = np.ascontiguousarray(inp["w_pb"][i])
    m["w_o_%d" % slot] = np.ascontiguousarray(inp["w_o"][i])
    m["g_ffn_%d" % slot] = _bc(inp["ffn_norm_g"][i])
    j = i // 2
    if kind == "dense":
        m["wg_%d" % slot] = np.ascontiguousarray(inp["dense_w_gate"][j])
        m["wu_%d" % slot] = np.ascontiguousarray(inp["dense_w_up"][j])
        m["wd_%d" % slot] = np.ascontiguousarray(inp["dense_w_down"][j])
    else:
        m["router_w_%d" % slot] = np.ascontiguousarray(
            np.transpose(inp["router_w"][j].reshape(KC, 128, NE), (1, 0, 2)).reshape(128, KC * NE))
        m["router_b_%d" % slot] = _bc(inp["router_b"][j])
        m["ewg_%d" % slot] = np.ascontiguousarray(inp["expert_w_gate"][j])
        m["ewu_%d" % slot] = np.ascontiguousarray(inp["expert_w_up"][j])
        m["ewd_%d" % slot] = np.ascontiguousarray(inp["expert_w_down"][j])
    return m


def _run(nc, shared, xfull, n=8):
    in_maps = []
    zeros = np.zeros((T, D), np.float32)
    for cid in range(n):
        b, half = cid // 2, cid % 2
        m = dict(shared)
        m.update(_consts(half))
        m["x_own"] = np.ascontiguousarray(xfull[b, half * T:(half + 1) * T])
        m["x_prev"] = np.ascontiguousarray(xfull[b, 0:T]) if half == 1 else zeros
        in_maps.append(m)
    res = run_bass_kernel_spmd(nc, in_maps, core_ids=list(range(n)))
    out = np.empty_like(xfull)
    for cid in range(n):
        b, half = cid // 2, cid % 2
        out[b, half * T:(half + 1) * T] = res.results[cid]["y"]
    return out, res


def kernel(**inp):
    inp = {k: np.asarray(v) for k, v in inp.items()}
    x = np.ascontiguousarray(inp["x"], dtype=np.float32)
    nc0 = build([(0, "dense")], final_norm=False)
    x1, _ = _run(nc0, _layer_inputs(inp, 0, 0, "dense"), x)
    nc1 = build([(0, "moe")], final_norm=True)
    sh = _layer_inputs(inp, 1, 0, "moe")
    sh["g_fin"] = _bc(inp["final_norm_g"])
    y, _ = _run(nc1, sh, x1)
    return y.astype(np.float32)
```
